# Optimizing a Trainium2 kernel written in Bass

```python
import math
import jax
import jax.numpy as jnp
from jax import lax
import numpy as np

D_MODEL = 1024
BATCH = 16
SEQ = 256
DEPTH = 4
DEC_BATCH = 8
DEC_SEQ = 4096
PAST_LEN = 512

GRID_W = 64
HEAD_DIM = 64
A_HEADS = 4
A_QK_DIM = 32
B_HEADS = 6
B_KV_HEADS = 2
C_HEADS = 6
C_KV_HEADS = 2
WINDOW = 128
Q_BLOCK = 128
ROPE_THETA = 10000.0
MIX_WIDTH = (A_HEADS + B_HEADS + C_HEADS) * HEAD_DIM
IN_WIDTHS = (A_HEADS * 2 * A_QK_DIM, A_HEADS * 2 * A_QK_DIM, A_HEADS * HEAD_DIM,
             B_HEADS * HEAD_DIM, B_KV_HEADS * HEAD_DIM, B_KV_HEADS * HEAD_DIM,
             C_HEADS * HEAD_DIM, C_KV_HEADS * HEAD_DIM, C_KV_HEADS * HEAD_DIM)
IN_WIDTH = sum(IN_WIDTHS)
N_EXPERTS = 32
TOP_K = 4
D_FF = D_MODEL
SWIGLU_ALPHA = 1.702
SWIGLU_LIMIT = 7.0
MOE_BLOCK = 256
LN_EPS = 1e-5
RMS_EPS = 1e-6
DEEPNORM_ALPHA = (2 * DEPTH) ** 0.25
DEEPNORM_BETA = (8 * DEPTH) ** -0.25
NEG_INF = -1e30

kernel_name = 'hybrid_flow_trunk_step'


def layer_norm(x, g, b):
    xf = x.astype(jnp.float32)
    mu = jnp.mean(xf, -1, keepdims=True)
    var = jnp.mean(jnp.square(xf - mu), -1, keepdims=True)
    return ((xf - mu) * lax.rsqrt(var + LN_EPS) * g + b).astype(x.dtype)


def rms_norm(x, g):
    xf = x.astype(jnp.float32)
    return (xf * lax.rsqrt(jnp.mean(xf * xf, -1, keepdims=True) + RMS_EPS) * g).astype(x.dtype)


def lambda_init(layer):
    return 0.8 - 0.6 * math.exp(-0.3 * layer)


def rope_tables(rows, dim):
    row = jnp.repeat(jnp.arange(rows), GRID_W).astype(jnp.float32)
    col = jnp.tile(jnp.arange(GRID_W), rows).astype(jnp.float32)
    nf = dim // 4
    freqs = ROPE_THETA ** (-jnp.arange(nf, dtype=jnp.float32) / nf)
    ang = jnp.concatenate([row[:, None] * freqs, col[:, None] * freqs], -1)
    return jnp.cos(ang), jnp.sin(ang)


def apply_rope_2d(x, cos, sin):
    q = x.shape[-1] // 4
    xf = x.astype(jnp.float32)
    parts = []
    for axis in range(2):
        xa = xf[..., axis * 2 * q:(axis + 1) * 2 * q]
        x1, x2 = xa[..., :q], xa[..., q:]
        c = cos[:, None, axis * q:(axis + 1) * q]
        s = sin[:, None, axis * q:(axis + 1) * q]
        parts += [x1 * c - x2 * s, x1 * s + x2 * c]
    return jnp.concatenate(parts, -1).astype(x.dtype)


def rope_diff(x, cos, sin):
    b, s, h, d = x.shape
    return apply_rope_2d(x.reshape(b, s, 2 * h, d // 2), cos, sin).reshape(b, s, h, d)


def map_query_blocks(fn, q):
    b, s = q.shape[:2]
    nb = s // Q_BLOCK
    blocks = jnp.moveaxis(q.reshape(b, nb, Q_BLOCK, *q.shape[2:]), 1, 0)
    out = lax.map(lambda args: fn(args[0], args[1]), (jnp.arange(nb), blocks))
    return jnp.moveaxis(out, 0, 1).reshape(b, s, *out.shape[3:])


def gqa_attend(q, k, v, sink=None, mask=None):
    b, nq, hq, d = q.shape
    hk = k.shape[2]
    g = hq // hk
    qg = q.reshape(b, nq, hk, g, d)
    s = jnp.einsum('bqhgd,bkhd->bhgqk', qg, k, preferred_element_type=jnp.float32) * (d ** -0.5)
    if mask is not None:
        s = jnp.where(mask, s, NEG_INF)
    if sink is not None:
        sink_col = jnp.broadcast_to(sink.astype(jnp.float32).reshape(1, hk, g, 1, 1), s.shape[:-1] + (1,))
        p = jax.nn.softmax(jnp.concatenate([s, sink_col], -1), axis=-1)[..., :-1]
    else:
        p = jax.nn.softmax(s, axis=-1)
    o = jnp.einsum('bhgqk,bkhd->bqhgd', p.astype(v.dtype), v)
    return o.reshape(b, nq, hq, v.shape[-1])


def diff_attend(q, k, v, lam):
    q1, q2 = jnp.split(q, 2, axis=-1)
    k1, k2 = jnp.split(k, 2, axis=-1)
    scale = A_QK_DIM ** -0.5
    s1 = jnp.einsum('bqhd,bkhd->bhqk', q1, k1, preferred_element_type=jnp.float32) * scale
    s2 = jnp.einsum('bqhd,bkhd->bhqk', q2, k2, preferred_element_type=jnp.float32) * scale
    p = jax.nn.softmax(s1, axis=-1) - lam * jax.nn.softmax(s2, axis=-1)
    return jnp.einsum('bhqk,bkhd->bqhd', p.astype(v.dtype), v)


def window_attend_latent(q, k, v, k_ctx, v_ctx, sink):
    s = q.shape[1]
    pad = ((0, 0), (WINDOW, WINDOW), (0, 0), (0, 0))
    kp = jnp.pad(k, pad)
    vp = jnp.pad(v, pad)
    span = Q_BLOCK + 2 * WINDOW
    qi = jnp.arange(Q_BLOCK)
    kj = jnp.arange(span) - WINDOW
    ctx_mask = jnp.ones((Q_BLOCK, k_ctx.shape[1]), dtype=bool)

    def block(i, qb):
        kb = lax.dynamic_slice_in_dim(kp, i * Q_BLOCK, span, axis=1)
        vb = lax.dynamic_slice_in_dim(vp, i * Q_BLOCK, span, axis=1)
        qpos = i * Q_BLOCK + qi
        kpos = i * Q_BLOCK + kj
        band = (jnp.abs(qpos[:, None] - kpos[None, :]) <= WINDOW) & (kpos[None, :] >= 0) & (kpos[None, :] < s)
        mask = jnp.concatenate([band, ctx_mask], axis=-1)
        return gqa_attend(qb, jnp.concatenate([kb, k_ctx], 1), jnp.concatenate([vb, v_ctx], 1), sink, mask)

    return map_query_blocks(block, q)


def clamped_swiglu(gu):
    glu, lin = jnp.split(gu, 2, axis=-1)
    glu = jnp.minimum(glu, SWIGLU_LIMIT)
    lin = jnp.clip(lin, -SWIGLU_LIMIT, SWIGLU_LIMIT)
    return glu * jax.nn.sigmoid(SWIGLU_ALPHA * glu) * (lin + 1)


def moe_ffn(h, p):
    b, s, d = h.shape
    n_tok = b * s
    t = h.reshape(n_tok, d)
    logits = jnp.dot(t, p['w_router'], preferred_element_type=jnp.float32) + p['b_router'].astype(jnp.float32)
    top_val, top_idx = lax.top_k(logits, TOP_K)
    gates = jax.nn.softmax(top_val, axis=-1)
    n_asg = n_tok * TOP_K
    flat_e = top_idx.reshape(-1)
    order = jnp.argsort(flat_e)
    sorted_e = flat_e[order]
    counts = jnp.bincount(flat_e, length=N_EXPERTS)
    starts = jnp.cumsum(counts) - counts
    padded = (counts + MOE_BLOCK - 1) // MOE_BLOCK * MOE_BLOCK
    pends = jnp.cumsum(padded)
    pstarts = pends - padded
    dest_sorted = (pstarts[sorted_e] + jnp.arange(n_asg) - starts[sorted_e]).astype(jnp.int32)
    dest = jnp.zeros((n_asg,), jnp.int32).at[order].set(dest_sorted)
    n_blocks = -(-n_asg // MOE_BLOCK) + N_EXPERTS
    buf = jnp.zeros((n_blocks * MOE_BLOCK, d), h.dtype).at[dest].set(t[jnp.arange(n_asg) // TOP_K])
    block_e = jnp.minimum(jnp.searchsorted(pends, jnp.arange(n_blocks) * MOE_BLOCK, side='right'), N_EXPERTS - 1)

    def expert_block(args):
        xb, e = args
        gu = xb @ p['w_gate_up'][e] + p['b_gate_up'][e]
        return clamped_swiglu(gu) @ p['w_down'][e] + p['b_down'][e]

    out = lax.map(expert_block, (buf.reshape(n_blocks, MOE_BLOCK, d), block_e)).reshape(-1, d)
    y = jnp.sum(out[dest].reshape(n_tok, TOP_K, d) * gates[..., None].astype(out.dtype), axis=1)
    return y.astype(h.dtype).reshape(b, s, d)


def split_points():
    pts, acc = [], 0
    for w in IN_WIDTHS[:-1]:
        acc += w
        pts.append(acc)
    return pts


def trunk_layer(x, cond, p, lam_init, ctx_kv=None, rope=None):
    bsz, s = x.shape[:2]
    mod = jax.nn.silu(cond) @ p['w_mod'] + p['b_mod']
    shift1, scale1, gate1, shift2, scale2, gate2 = [m[:, None, :] for m in jnp.split(mod, 6, axis=-1)]
    h = x * (1 + scale1) + shift1
    proj = h @ p['w_in']
    qa, ka, va, qb, kb, vb, qc, kc, vc = jnp.split(proj, split_points(), axis=-1)
    heads = lambda a, n: a.reshape(bsz, s, n, -1)
    qa, ka, va = heads(qa, A_HEADS), heads(ka, A_HEADS), heads(va, A_HEADS)
    qb = rms_norm(heads(qb, B_HEADS), p['b_q_norm_g'])
    kb = rms_norm(heads(kb, B_KV_HEADS), p['b_k_norm_g'])
    vb = heads(vb, B_KV_HEADS)
    qc, kc, vc = heads(qc, C_HEADS), heads(kc, C_KV_HEADS), heads(vc, C_KV_HEADS)
    lam_vec = p['a_lambda'].astype(jnp.float32)
    lam = jnp.exp(jnp.sum(lam_vec[0] * lam_vec[1])) - jnp.exp(jnp.sum(lam_vec[2] * lam_vec[3])) + lam_init
    if ctx_kv is None:
        new_kv = (ka, va, kb, vb, kc, vc)
        oa = map_query_blocks(lambda i, q: diff_attend(q, ka, va, lam), qa)
        ob = map_query_blocks(lambda i, q: gqa_attend(q, kb, vb), qb)
        oc = map_query_blocks(lambda i, q: gqa_attend(q, kc, vc, sink=p['c_sink']), qc)
    else:
        new_kv = None
        ka_x, va_x, kb_x, vb_x, kc_x, vc_x = ctx_kv
        (cos64, sin64), (cos32, sin32) = rope
        qa, ka = rope_diff(qa, cos32, sin32), rope_diff(ka, cos32, sin32)
        qb, kb = apply_rope_2d(qb, cos64, sin64), apply_rope_2d(kb, cos64, sin64)
        qc, kc = apply_rope_2d(qc, cos64, sin64), apply_rope_2d(kc, cos64, sin64)
        ka_all, va_all = jnp.concatenate([ka, ka_x], 1), jnp.concatenate([va, va_x], 1)
        kb_all, vb_all = jnp.concatenate([kb, kb_x], 1), jnp.concatenate([vb, vb_x], 1)
        oa = map_query_blocks(lambda i, q: diff_attend(q, ka_all, va_all, lam), qa)
        ob = map_query_blocks(lambda i, q: gqa_attend(q, kb_all, vb_all), qb)
        oc = window_attend_latent(qc, kc, vc, kc_x, vc_x, p['c_sink'])
    oa = rms_norm(oa, p['a_subln_g']) * (1.0 - lam_init)
    mixed = jnp.concatenate([oa.reshape(bsz, s, -1), ob.reshape(bsz, s, -1), oc.reshape(bsz, s, -1)], axis=-1)
    o = mixed @ p['w_out']
    x = layer_norm(DEEPNORM_ALPHA * x + gate1 * o, p['ln1_g'], p['ln1_b'])
    h2 = x * (1 + scale2) + shift2
    x = layer_norm(DEEPNORM_ALPHA * x + gate2 * moe_ffn(h2, p), p['ln2_g'], p['ln2_b'])
    return x, new_kv


def setup_inputs(seed: int = 0) -> dict:
    key = jax.random.key(seed)
    ks = jax.random.split(key, 32)
    L = DEPTH

    def nrm(k, shape, scale):
        return jax.random.normal(k, shape, jnp.float32) * scale

    return {
        'x_prompt': nrm(ks[0], (BATCH, SEQ, D_MODEL), 1.0),
        'x_sample': nrm(ks[1], (DEC_BATCH, DEC_SEQ, D_MODEL), 1.0),
        'cache_a_k': nrm(ks[2], (DEC_BATCH, L, PAST_LEN, A_HEADS, 2 * A_QK_DIM), 1.0),
        'cache_a_v': nrm(ks[3], (DEC_BATCH, L, PAST_LEN, A_HEADS, HEAD_DIM), 1.0),
        'cache_b_k': nrm(ks[4], (DEC_BATCH, L, PAST_LEN, B_KV_HEADS, HEAD_DIM), 1.0),
        'cache_b_v': nrm(ks[5], (DEC_BATCH, L, PAST_LEN, B_KV_HEADS, HEAD_DIM), 1.0),
        'cache_c_k': nrm(ks[6], (DEC_BATCH, L, PAST_LEN, C_KV_HEADS, HEAD_DIM), 1.0),
        'cache_c_v': nrm(ks[7], (DEC_BATCH, L, PAST_LEN, C_KV_HEADS, HEAD_DIM), 1.0),
        'c': nrm(ks[8], (DEC_BATCH, D_MODEL), 1.0),
        'c_ctx': nrm(ks[9], (D_MODEL,), 1.0),
        'w_mod': nrm(ks[10], (L, D_MODEL, 6 * D_MODEL), 0.5 * D_MODEL ** -0.5),
        'b_mod': nrm(ks[11], (L, 6 * D_MODEL), 0.02),
        'w_in': nrm(ks[12], (L, D_MODEL, IN_WIDTH), D_MODEL ** -0.5),
        'a_lambda': nrm(ks[13], (L, 4, A_QK_DIM), 0.1),
        'a_subln_g': 1.0 + nrm(ks[14], (L, HEAD_DIM), 0.02),
        'b_q_norm_g': 1.0 + nrm(ks[15], (L, HEAD_DIM), 0.02),
        'b_k_norm_g': 1.0 + nrm(ks[16], (L, HEAD_DIM), 0.02),
        'c_sink': nrm(ks[17], (L, C_HEADS), 0.5),
        'w_out': nrm(ks[18], (L, MIX_WIDTH, D_MODEL), DEEPNORM_BETA * MIX_WIDTH ** -0.5),
        'ln1_g': 1.0 + nrm(ks[19], (L, D_MODEL), 0.02),
        'ln1_b': nrm(ks[20], (L, D_MODEL), 0.02),
        'w_router': nrm(ks[21], (L, D_MODEL, N_EXPERTS), D_MODEL ** -0.5),
        'b_router': nrm(ks[22], (L, N_EXPERTS), 0.01),
        'w_gate_up': nrm(ks[23], (L, N_EXPERTS, D_MODEL, 2 * D_FF), D_MODEL ** -0.5),
        'b_gate_up': nrm(ks[24], (L, N_EXPERTS, 2 * D_FF), 0.02),
        'w_down': nrm(ks[25], (L, N_EXPERTS, D_FF, D_MODEL), DEEPNORM_BETA * D_FF ** -0.5),
        'b_down': nrm(ks[26], (L, N_EXPERTS, D_MODEL), 0.02),
        'ln2_g': 1.0 + nrm(ks[27], (L, D_MODEL), 0.02),
        'ln2_b': nrm(ks[28], (L, D_MODEL), 0.02),
    }


def reference(x_prompt, x_sample, cache_a_k, cache_a_v, cache_b_k, cache_b_v, cache_c_k, cache_c_v,
              c, c_ctx, w_mod, b_mod, w_in, a_lambda, a_subln_g, b_q_norm_g, b_k_norm_g, c_sink,
              w_out, ln1_g, ln1_b, w_router, b_router, w_gate_up, b_gate_up, w_down, b_down,
              ln2_g, ln2_b):
    layers = [dict(w_mod=w_mod[l], b_mod=b_mod[l], w_in=w_in[l], a_lambda=a_lambda[l],
                   a_subln_g=a_subln_g[l], b_q_norm_g=b_q_norm_g[l], b_k_norm_g=b_k_norm_g[l],
                   c_sink=c_sink[l], w_out=w_out[l], ln1_g=ln1_g[l], ln1_b=ln1_b[l],
                   w_router=w_router[l], b_router=b_router[l], w_gate_up=w_gate_up[l],
                   b_gate_up=b_gate_up[l], w_down=w_down[l], b_down=b_down[l],
                   ln2_g=ln2_g[l], ln2_b=ln2_b[l]) for l in range(DEPTH)]

    y = x_prompt
    kv_layers = []
    for l in range(DEPTH):
        y, kv = trunk_layer(y, c_ctx[None, :], layers[l], lambda_init(l))
        kv_layers.append(kv)
    new_a_k = jnp.stack([kv[0] for kv in kv_layers], axis=1)
    new_a_v = jnp.stack([kv[1] for kv in kv_layers], axis=1)
    new_b_k = jnp.stack([kv[2] for kv in kv_layers], axis=1)
    new_b_v = jnp.stack([kv[3] for kv in kv_layers], axis=1)
    new_c_k = jnp.stack([kv[4] for kv in kv_layers], axis=1)
    new_c_v = jnp.stack([kv[5] for kv in kv_layers], axis=1)

    rows = x_sample.shape[1] // GRID_W
    rope = (rope_tables(rows, HEAD_DIM), rope_tables(rows, A_QK_DIM))
    z = x_sample
    for l in range(DEPTH):
        ctx_kv = (cache_a_k[:, l], cache_a_v[:, l], cache_b_k[:, l], cache_b_v[:, l],
                  cache_c_k[:, l], cache_c_v[:, l])
        z, _ = trunk_layer(z, c, layers[l], lambda_init(l), ctx_kv=ctx_kv, rope=rope)

    return (y, z, new_a_k, new_a_v, new_b_k, new_b_v, new_c_k, new_c_v)
```

```python
import contextlib
import math
import os
import numpy as np
import ml_dtypes
import concourse.bass as bass
import concourse.mybir as mybir
from concourse.bass_utils import run_bass_kernel_spmd

F32 = mybir.dt.float32
BF16 = mybir.dt.bfloat16
I32 = mybir.dt.int32
U32 = mybir.dt.uint32
ALU = mybir.AluOpType
AF = mybir.ActivationFunctionType
AX = mybir.AxisListType
ET = mybir.EngineType

P = 128
D = 1024
DEPTH = 4
NCORES = 8
SEQ = 256
DEC_SEQ = 4096
PAST = 512
NT_P = 4
NT_S = 32
NT = NT_P + NT_S
NTOK = NT * P
NE = 32
TOPK = 4
BLK = 256
NBLK = (NTOK * TOPK) // BLK + NE
NROWS = NBLK * BLK
LN_EPS = 1e-5
RMS_EPS = 1e-6
ALPHA = (2 * DEPTH) ** 0.25
SW_ALPHA = 1.702
SW_LIM = 7.0
GRID_W = 64
THETA = 10000.0
QA, KA, VA, QB, KB, VB_, QC, KC, VC = 0, 256, 512, 768, 1152, 1280, 1408, 1792, 1920


KDBG = int(os.environ.get('KDBG', '9'))
KSUB = int(os.environ.get('KSUB', '9'))
KS3 = int(os.environ.get('KS3', '9'))


def lambda_init(layer):
    return 0.8 - 0.6 * math.exp(-0.3 * layer)


class Buf:
    __slots__ = ("name", "w", "r", "excl")

    def __init__(self, name, excl=False):
        self.name = name
        self.excl = excl
        self.w = None
        self.r = {}


class DSem:
    def __init__(self, sched, name):
        self.sem = sched.es.enter_context(sched.nc.semaphore(name))
        self.cum = 0
        self.key = ("d", name)
        sched.sems[self.key] = self


class Sched:
    ENG = ("pe", "act", "dve", "pool", "sp")

    def __init__(self, nc, es):
        self.nc = nc
        self.es = es
        self.ops = {e: [] for e in self.ENG}
        self.cnt = {e: 0 for e in self.ENG}
        self.sems = {}
        self.esem = {}
        for e in self.ENG:
            self.esem[e] = es.enter_context(nc.semaphore("s_" + e))
        self.seen = {e: {} for e in self.ENG}
        self.dsems = []
        self.ndma = 0
        self.pool = {}
        self.assign = {}

    def dsem(self, name):
        d = DSem(self, name)
        self.dsems.append(d)
        return d

    def _semobj(self, key):
        return self.esem[key[1]] if key[0] == "e" else self.sems[key].sem

    def _waits(self, eng, r, w):
        need = {}

        def add(tok):
            if tok is None:
                return
            k, v = tok
            if k == ("e", eng) and eng == "pe":
                return
            if need.get(k, 0) < v:
                need[k] = v
        for b in r:
            add(b.w)
            if b.excl:
                for k, v in b.r.items():
                    if k != ("e", eng):
                        add((k, v))
        for b in w:
            add(b.w)
            for k, v in b.r.items():
                add((k, v))
        out = []
        seen = self.seen[eng]
        for k, v in need.items():
            if seen.get(k, 0) >= v:
                continue
            seen[k] = v
            out.append((self._semobj(k), v))
        return out

    def _mark(self, tok, r, w):
        for b in w:
            b.w = tok
            b.r = {}
        for b in r:
            if b.r.get(tok[0], 0) < tok[1]:
                b.r[tok[0]] = tok[1]

    def op(self, eng, fn, r=(), w=()):
        waits = self._waits(eng, r, w)
        self.cnt[eng] += 1
        tok = (("e", eng), self.cnt[eng])
        self.ops[eng].append((waits, fn, (self.esem[eng], 1)))
        self._mark(tok, r, w)
        return tok

    def raw(self, eng, fn, r=()):
        waits = self._waits(eng, r, ())
        self.ops[eng].append((waits, fn, None))

    def ds_for(self, buf, eng):
        kind = "sw" if eng == "pool" else "hw"
        key = (id(buf), kind)
        d = self.assign.get(key)
        if d is None:
            pool = self.pool.setdefault(kind, [])
            n = sum(1 for k in self.assign if k[1] == kind)
            if len(pool) <= n:
                pool.append(self.dsem("d_%s%d" % (kind, len(pool))))
            d = pool[n]
            self.assign[key] = d
        return d

    def dma(self, eng, fn, ds, r=(), w=()):
        bufs = list(r) + list(w)
        assert len(bufs) == 1, "a DMA names exactly one SBUF-side buffer"
        ds = self.ds_for(bufs[0], eng)
        waits = self._waits(eng, r, w)
        ds.cum += 16
        tok = (ds.key, ds.cum)
        self.ops[eng].append((waits, fn, (ds.sem, 16)))
        self._mark(tok, r, w)
        self.ndma += 1
        return tok

    def barrier(self):
        for eng in self.ENG:
            waits = []
            seen = self.seen[eng]
            for e2 in self.ENG:
                k = ("e", e2)
                v = self.cnt[e2]
                if e2 != eng and v > 0 and seen.get(k, 0) < v:
                    seen[k] = v
                    waits.append((self.esem[e2], v))
            for d in self.dsems:
                if d.cum > 0 and seen.get(d.key, 0) < d.cum:
                    seen[d.key] = d.cum
                    waits.append((d.sem, d.cum))
            if waits:
                self.ops[eng].append((waits, None, None))
        self.assign = {}
        with self.nc.Block() as block:
            self.emit(block)
        self.ops = {e: [] for e in self.ENG}

    def emit(self, block):
        nc = self.nc

        def replay(name):
            def run(e):
                for waits, fn, inc in self.ops[name]:
                    for s, v in waits:
                        e.wait_ge(s, v)
                    if fn is None:
                        continue
                    ins = fn(e)
                    if inc is not None:
                        ins.then_inc(inc[0], inc[1])
            return run
        block.tensor(replay("pe"))
        block.scalar(replay("act"))
        block.vector(replay("dve"))
        block.gpsimd(replay("pool"))
        block.sync(replay("sp"))


def _consts():
    c = {}
    c["ident_bf"] = np.eye(P, dtype=np.float32).astype(ml_dtypes.bfloat16)
    c["ident_f"] = np.eye(P, dtype=np.float32)
    tri = np.zeros((P, P), np.float32)
    for t in range(P):
        tri[:t, t] = 1.0
    c["tri_f"] = tri
    c["ones_f"] = np.ones((P, P), np.float32)
    blk = np.zeros((P, P), np.float32)
    blk[:64, :64] = 1.0
    blk[64:, 64:] = 1.0
    c["blk64_f"] = blk
    tri32 = np.zeros((NE, NE), np.float32)
    for e in range(NE):
        tri32[:e, e] = 1.0
    c["tri32_f"] = tri32
    inc32 = np.zeros((NE, NE), np.float32)
    for e in range(NE):
        inc32[:e + 1, e] = 1.0
    c["inc32_f"] = inc32
    def rotm(q):
        m = np.zeros((P, P), np.float32)
        for f in range(P):
            j = f % (2 * q)
            if j < q:
                m[f + q, f] = -1.0
            else:
                m[f - q, f] = 1.0
        return m
    c["rot64_f"] = rotm(16)
    c["rot32_f"] = rotm(8)
    t = np.arange(DEC_SEQ)
    pos = np.stack([(t // GRID_W).astype(np.float32), (t % GRID_W).astype(np.float32)])
    def tables(hd):
        nf = hd // 4
        freqs = (np.float32(THETA) ** (-np.arange(nf, dtype=np.float32) / np.float32(nf))).astype(np.float32)
        cs = np.zeros((P, DEC_SEQ), np.float32)
        sn = np.zeros((P, DEC_SEQ), np.float32)
        for p_ in range(P):
            d = p_ % hd
            axis = d // (hd // 2)
            j = d % nf
            ang = (pos[axis] * freqs[j]).astype(np.float32)
            cs[p_] = np.cos(ang)
            sn[p_] = np.sin(ang)
        return cs, sn
    c["cos64"], c["sin64"] = tables(64)
    c["cos32"], c["sin32"] = tables(32)
    wm = np.zeros((6, P, 512), np.float32)
    kk = np.arange(P)[:, None]
    qq = np.arange(P)[None, :]
    for r in range(6):
        for b in range(4):
            rel = r - 1 - b
            if rel == 0:
                wm[r, :, b * P:(b + 1) * P] = 1.0
            elif rel == -1:
                wm[r, :, b * P:(b + 1) * P] = (qq <= kk)
            elif rel == 1:
                wm[r, :, b * P:(b + 1) * P] = (kk <= qq)
    c["wmask"] = wm.astype(ml_dtypes.bfloat16)
    mc = np.zeros((P, 2), np.float32)
    mc[:, 0] = (np.arange(P) % 64) < 32
    mc[:, 1] = (np.arange(P) % 64) >= 32
    c["mcol"] = mc
    c["kpc"] = (np.arange(8, dtype=np.float32)[None, :] * P + np.arange(P, dtype=np.float32)[:, None]).astype(np.float32)
    c["iota_e"] = np.tile(np.arange(NE, dtype=np.float32)[None, :], (P, 1))
    c["blkstart"] = np.tile((np.arange(NBLK, dtype=np.float32) * BLK)[None, :], (NE, 1))
    return c


CONST_SPECS = None


def build(depth_run=DEPTH, stop_after=None):
    nc = bass.Bass("TRN2", target_bir_lowering=False)
    es = contextlib.ExitStack()
    S = Sched(nc, es)
    consts = _consts()

    def din(name, shape, dt=F32):
        return nc.dram_tensor(name, list(shape), dt, kind="ExternalInput").ap()

    def dout(name, shape, dt=F32):
        return nc.dram_tensor(name, list(shape), dt, kind="ExternalOutput").ap()

    def dint(name, shape, dt=F32):
        return nc.dram_tensor(name, list(shape), dt, kind="Internal").ap()

    L = depth_run
    xp = din("xp", [NT_P * P, D])
    xs = din("xs", [NT_S * P, D])
    cak = din("cak", [L, PAST, 256])
    cav = din("cav", [L, PAST, 256])
    cbk = din("cbk", [L, PAST, 128])
    cbv = din("cbv", [L, PAST, 128])
    cck = din("cck", [L, PAST, 128])
    ccv = din("ccv", [L, PAST, 128])
    cond2 = din("cond2", [2, D])
    w_mod = din("w_mod", [L, D, 6 * D])
    b_mod = din("b_mod", [L, 6 * D])
    w_in = din("w_in", [L, D, 2048])
    a_lambda = din("a_lambda", [L, 4 * 32])
    a_subln_g = din("a_subln_g", [L, 64])
    b_q_norm_g = din("b_q_norm_g", [L, 64])
    b_k_norm_g = din("b_k_norm_g", [L, 64])
    c_sink = din("c_sink", [L, 6])
    w_out = din("w_out", [L, D, D])
    ln1_g = din("ln1_g", [L, D])
    ln1_b = din("ln1_b", [L, D])
    w_router = din("w_router", [L, D, NE])
    b_router = din("b_router", [L, NE])
    w_gate_up = din("w_gate_up", [L, NE, D, 2 * D])
    b_gate_up = din("b_gate_up", [L, NE, 2 * D])
    w_down = din("w_down", [L, NE, D, D])
    b_down = din("b_down", [L, NE, D])
    ln2_g = din("ln2_g", [L, D])
    ln2_b = din("ln2_b", [L, D])
    cin = {}
    for k, v in consts.items():
        cin[k] = din("c_" + k, v.shape, BF16 if v.dtype == ml_dtypes.bfloat16 else F32)

    yp = dout("yp", [NT_P * P, D])
    ys = dout("ys", [NT_S * P, D])
    nak = dout("nak", [2, DEPTH, SEQ, 256])
    nav = dout("nav", [2, DEPTH, SEQ, 256])
    nbk = dout("nbk", [2, DEPTH, SEQ, 128])
    nbv = dout("nbv", [2, DEPTH, SEQ, 128])
    nck = dout("nck", [2, DEPTH, SEQ, 128])
    ncv = dout("ncv", [2, DEPTH, SEQ, 128])

    xres = dint("xres", [NTOK, D])
    x1d = dint("x1d", [NTOK, D])
    modd = dint("modd", [DEPTH, 2, 6 * D])
    h2d = dint("h2d", [NTOK, D], BF16)
    hTd = dint("hTd", [8, P, NTOK], BF16)
    ktd = dint("ktd", [4, P, NTOK], BF16)
    vbd = dint("vbd", [NT, P, 512], BF16)
    mixTd = dint("mixTd", [8, P, NTOK], BF16)
    mbuf = dint("mbuf", [NROWS, D], BF16)
    obuf = dint("obuf", [NROWS, D])

    uid = [0]

    def T(stack, name, shape, dt=F32):
        uid[0] += 1
        name = "%s_%d" % (name, uid[0])
        t = stack.enter_context(nc.sbuf_tensor(name, list(shape), dt))
        return t, Buf(name)

    def mm(out, lhsT, rhs, start, stop, r, w):
        return S.op("pe", lambda e: e.matmul(out, lhsT=lhsT, rhs=rhs, start=start, stop=stop), r, w)

    def tp(out, in_, ident, r, w):
        return S.op("pe", lambda e: e.transpose(out, in_, ident), r, w)

    def act(out, in_, func, r, w, bias=None, scale=None, accum=None):
        kw = {}
        if bias is not None:
            kw["bias"] = bias
        if scale is not None:
            kw["scale"] = scale
        if accum is not None:
            kw["accum_out"] = accum
        return S.op("act", lambda e: e.activation(out, in_, func, **kw), r, w)

    def tt(out, a, b, op, r, w, eng="dve"):
        return S.op(eng, lambda e: e.tensor_tensor(out, a, b, op), r, w)

    def ts(out, a, s1, s2, op0, op1, r, w, eng="dve"):
        if op1 is None:
            return S.op(eng, lambda e: e.tensor_scalar(out, a, s1, None, op0), r, w)
        return S.op(eng, lambda e: e.tensor_scalar(out, a, s1, s2, op0, op1), r, w)

    def stt(out, a, s, b, op0, op1, r, w, eng="dve"):
        return S.op(eng, lambda e: e.scalar_tensor_tensor(out, a, s, b, op0, op1), r, w)

    def cp(out, in_, r, w, eng="dve"):
        if eng == "act":
            return S.op("act", lambda e: e.activation(out, in_, AF.Copy), r, w)
        return S.op(eng, lambda e: e.tensor_copy(out, in_), r, w)

    def rsqrt_(ap, B):
        act(ap, ap, AF.Ln, [B], [B])
        act(ap, ap, AF.Exp, [B], [B], scale=-0.5)

    def dma(q, out, in_, ds, r, w, **kw):
        return S.dma(q, lambda e: e.dma_start(out=out, in_=in_, **kw), ds, r, w)

    top = es
    ps_all = top.enter_context(nc.psum_tensor("ps_all", [P, 8 * 512], F32))
    PSB = [Buf("psb%d" % i, excl=True) for i in range(8)]

    def psf(i, n=512, off=0):
        return ps_all[:, i * 512 + off: i * 512 + off + n]

    def psbf(i, nbanks=1):
        return ps_all[:, i * 512:(i + nbanks) * 512].bitcast(BF16)

    csb = {}
    cbuf = Buf("consts")
    ds_c = None
    for k in ("ident_bf", "ident_f", "tri_f", "ones_f", "blk64_f", "rot64_f", "rot32_f", "iota_e", "mcol", "kpc"):
        t, _ = T(top, "k_" + k, consts[k].shape, BF16 if consts[k].dtype == ml_dtypes.bfloat16 else F32)
        csb[k] = t
        dma("sp", t[:], cin[k], ds_c, [], [cbuf])
    for k in ("tri32_f", "inc32_f", "blkstart"):
        t, _ = T(top, "k_" + k, consts[k].shape, F32)
        csb[k] = t
        dma("sp", t[:], cin[k], ds_c, [], [cbuf])
    wmask, _ = T(top, "k_wmask", [P, 6, 512], BF16)
    dma("sp", wmask[:], cin["wmask"].rearrange("r p n -> p r n"), ds_c, [], [cbuf])
    ident_bf, ident_f = csb["ident_bf"], csb["ident_f"]

    lgall, lgB = T(top, "lgall", [P, NT, NE])
    top8all, t8B = T(top, "top8all", [P, NT, 8])
    rankall, rkB = T(top, "rankall", [P, NT, NE])
    tot, totB = T(top, "tot", [P, NE])
    desti, diB = T(top, "desti", [P, NT, TOPK], I32)
    gk, gkB = T(top, "gk", [P, NT, TOPK])
    widx, wiB = T(top, "widx", [P, NBLK, 8], I32)
    OH, ohB = T(top, "OH", [NE, NBLK], BF16)
    scT, scB = T(top, "scT", [P, 8, 2], BF16)

    ds_ld = ds_st = ds_w = ds_misc = [None] * 8
    ds_bc = None

    with contextlib.ExitStack() as st:
        cT, cTB = T(st, "cT", [P, 8, 2])
        sg, sgB = T(st, "sg", [P, 8, 2])
        zt, ztB = T(st, "zt", [P, 4, D], BF16)
        for j in range(2):
            for k in range(8):
                dma("sp", cT[:, k, j:j + 1], cond2[j, k * P:(k + 1) * P].rearrange("(p o) -> p o", o=1),
                    ds_misc[0], [], [cTB])
        act(sg[:], cT[:], AF.Sigmoid, [cTB], [sgB])
        tt(scT[:], cT[:], sg[:], ALU.mult, [cTB, sgB], [scB])
        S.op("dve", lambda e: e.memset(zt[:], 0.0), [], [ztB])
        mb4 = mbuf.rearrange("(n j p) d -> n p j d", p=P, j=4)
        for n in range(NROWS // (4 * P)):
            dma("sp", mb4[n], zt[:], ds_misc[1], [ztB], [])
        S.barrier()

    def x_src(l, i):
        if l == 0:
            return xp[i * P:(i + 1) * P, :] if i < NT_P else xs[(i - NT_P) * P:(i - NT_P + 1) * P, :]
        return xres[i * P:(i + 1) * P, :]

    def x_dst(l, i):
        if l == DEPTH - 1:
            return yp[i * P:(i + 1) * P, :] if i < NT_P else ys[(i - NT_P) * P:(i - NT_P + 1) * P, :]
        return xres[i * P:(i + 1) * P, :]

    def bcast_row(ap_row, n):
        return ap_row.to_broadcast([P, n])

    def layer_norm(st_pool, xa, xaB, gt, bt, gbB, bbB, out, outB, tag):
        stats, sB = st_pool[tag + "stats"]
        mv, mB = st_pool[tag + "mv"]
        for hh in range(2):
            S.op("dve", lambda e, hh=hh: e.bn_stats(stats[:, hh, :], xa[:, hh * 512:(hh + 1) * 512]), [xaB], [sB])
        S.op("dve", lambda e: e.bn_aggr(mv[:], stats[:].rearrange("p a b -> p (a b)")), [sB], [mB])
        ts(mv[:, 1:2], mv[:, 1:2], LN_EPS, None, ALU.add, None, [mB], [mB])
        rsqrt_(mv[:, 1:2], mB)
        ts(xa[:], xa[:], mv[:, 0:1], mv[:, 1:2], ALU.subtract, ALU.mult, [xaB, mB], [xaB])
        tt(xa[:], xa[:], gt[:], ALU.mult, [xaB, gbB], [xaB])
        tt(out[:], xa[:], bt[:], ALU.add, [xaB, bbB], [outB])

    UNITS = [(0, 0, 2, False, False, 0), (0, 2, 2, False, False, 1), (1, 4, NT_S, True, True, None)]

    for l in range(depth_run):
        lam0 = lambda_init(l)
        with contextlib.ExitStack() as st:
            wm = [T(st, "wm%d" % i, [P, 8, 512], BF16) for i in range(2)]
            bm = [T(st, "bm%d" % i, [2, 512]) for i in range(2)]
            mr = [T(st, "mr%d" % i, [2, 512]) for i in range(2)]
            for n in range(12):
                sl = n % 2
                wt, wB = wm[sl]
                bt_, bB = bm[sl]
                mt_, mB_ = mr[sl]
                dma("pool", wt[:], w_mod[l, :, n * 512:(n + 1) * 512].rearrange("(k p) n -> p k n", p=P),
                    ds_w[sl], [], [wB])
                dma("sp", bt_[:], b_mod[l:l + 1, n * 512:(n + 1) * 512].to_broadcast([2, 512]), ds_ld[sl], [], [bB])
                for k in range(8):
                    mm(psf(sl)[0:2, :], scT[:, k, :], wt[:, k, :], k == 0, k == 7, [scB, wB], [PSB[sl]])
                tt(mt_[:], psf(sl)[0:2, :], bt_[:], ALU.add, [PSB[sl], bB], [mB_])
                if n in (2, 3, 8, 9):
                    ts(mt_[:], mt_[:], 1.0, None, ALU.add, None, [mB_], [mB_])
                dma("sp", modd[l, :, n * 512:(n + 1) * 512], mt_[:], ds_st[sl], [mB_], [])
            S.barrier()
        if stop_after == "mod" and l == depth_run - 1:
            break

        with contextlib.ExitStack() as st:
            wkv, wkvB = T(st, "wkv", [P, 8, 1024], BF16)
            sc1, sc1B = T(st, "sc1", [P, D])
            sh1, sh1B = T(st, "sh1", [P, D])
            gkb, gkbB = T(st, "gkb", [P, 64])
            gkc, gkcB = T(st, "gkc", [P, 1])
            xt = [T(st, "xt%d" % i, [P, D]) for i in range(2)]
            hb = [T(st, "hb%d" % i, [P, D], BF16) for i in range(2)]
            hT, hTB = T(st, "hT", [P, 8, 512], BF16)
            vt = [T(st, "vt%d" % i, [P, 512], BF16) for i in range(2)]
            vf = [T(st, "vf%d" % i, [P, 512]) for i in range(2)]
            kf = [T(st, "kf%d" % i, [P, 512]) for i in range(2)]
            ssq, ssqB = T(st, "ssq", [P, 2])
            junk, junkB = T(st, "junk", [P, 64])
            tab = [T(st, "tab%d" % i, [P, 512]) for i in range(4)]
            q32, q32B = T(st, "q32", [P, 512])
            sq, sqB = T(st, "sq", [P, 512])
            r1, r1B = T(st, "r1", [P, 512])
            t1, t1B = T(st, "t1", [P, 512])
            t2, t2B = T(st, "t2", [P, 512])
            ko = [T(st, "ko%d" % i, [P, 512], BF16) for i in range(2)]

            for (c0, w_, o0) in ((KA, 256, 0), (KB, 128, 256), (KC, 128, 384), (VA, 256, 512), (VB_, 128, 768), (VC, 128, 896)):
                dma("pool", wkv[:, :, o0:o0 + w_], w_in[l, :, c0:c0 + w_].rearrange("(k p) n -> p k n", p=P),
                    ds_w[0], [], [wkvB])
            dma("sp", gkb[:], b_k_norm_g[l:l + 1, :].to_broadcast([P, 64]), ds_bc, [], [gkbB])
            for hh in range(2):
                dma("sp", gkc[hh * 64:(hh + 1) * 64, :], b_k_norm_g[l, :].rearrange("(d o) -> d o", o=1), ds_bc, [], [gkcB])

            def qk_post(psb_i, N, norm, rope_kind, scale, gcol, gcolB, out_ap, outB):
                src = psf(psb_i, N)
                srcB = PSB[psb_i]
                if norm:
                    act(sq[:, :N], src, AF.Square, [srcB], [sqB])
                    mm(psf(2, N), csb["blk64_f"][:], sq[:, :N], True, True, [sqB, cbuf], [PSB[2]])
                    ts(r1[:, :N], psf(2, N), 1.0 / 64, RMS_EPS, ALU.mult, ALU.add, [PSB[2]], [r1B])
                    rsqrt_(r1[:, :N], r1B)
                    ts(r1[:, :N], r1[:, :N], gcol, None, ALU.mult, None, [r1B, gcolB], [r1B])
                    if rope_kind is None:
                        tt(out_ap, src, r1[:, :N], ALU.mult, [srcB, r1B], [outB])
                        return
                    tt(q32[:, :N], src, r1[:, :N], ALU.mult, [srcB, r1B], [q32B])
                else:
                    if rope_kind is None:
                        act(out_ap, src, AF.Copy, [srcB], [outB], scale=float(scale))
                        return
                    act(q32[:, :N], src, AF.Copy, [srcB], [q32B], scale=float(scale))
                rotm = csb["rot64_f"] if rope_kind == 64 else csb["rot32_f"]
                ct, ctB = tab[0] if rope_kind == 64 else tab[2]
                sn, snB = tab[1] if rope_kind == 64 else tab[3]
                mm(psf(3, N), rotm[:], q32[:, :N], True, True, [q32B, cbuf], [PSB[3]])
                tt(t1[:, :N], q32[:, :N], ct[:, :N], ALU.mult, [q32B, ctB], [t1B])
                tt(t2[:, :N], psf(3, N), sn[:, :N], ALU.mult, [PSB[3], snB], [t2B])
                tt(out_ap, t1[:, :N], t2[:, :N], ALU.add, [t1B, t2B], [outB])

            for (cj, t0, ntl, rope, has_cache, pseq) in (UNITS if KDBG >= 9 else UNITS[:1]):
                dma("sp", sc1[:], modd[l, cj:cj + 1, D:2 * D].to_broadcast([P, D]), ds_bc, [], [sc1B])
                dma("sp", sh1[:], modd[l, cj:cj + 1, 0:D].to_broadcast([P, D]), ds_bc, [], [sh1B])
                ngrp = (ntl + 3) // 4
                for g in range(ngrp):
                    tiles = list(range(t0 + 4 * g, min(t0 + 4 * g + 4, t0 + ntl)))
                    N = P * len(tiles)
                    tok0 = tiles[0] * P
                    key0 = (tiles[0] - t0) * P
                    if rope:
                        for ti, nm in enumerate(("cos64", "sin64", "cos32", "sin32")):
                            dma("sp", tab[ti][0][:, :N], cin[nm][:, key0:key0 + N], ds_ld[2], [], [tab[ti][1]])
                    for ii, i in enumerate(tiles):
                        sl = i % 2
                        xtt, xB = xt[sl]
                        hbt, hbB = hb[sl]
                        if KSUB < 1:
                            continue
                        dma("sp", xtt[:], x_src(l, i), ds_ld[sl], [], [xB])
                        tt(xtt[:], xtt[:], sc1[:], ALU.mult, [xB, sc1B], [xB])
                        tt(hbt[:], xtt[:], sh1[:], ALU.add, [xB, sh1B], [hbB])
                        if KSUB < 2:
                            continue
                        for k in range(8):
                            tp(psbf(6)[:, k * P:(k + 1) * P], hbt[:, k * P:(k + 1) * P], ident_bf[:], [hbB, cbuf], [PSB[6]])
                        if KSUB < 3:
                            continue
                        cp(hT[:, :, ii * P:(ii + 1) * P], psbf(6).rearrange("p (k t) -> p k t", t=P), [PSB[6]], [hTB], eng="act")
                        if KDBG < 2:
                            continue
                        for k in range(8):
                            mm(psf(4), hT[:, k, ii * P:(ii + 1) * P], wkv[:, k, 512:1024], k == 0, k == 7, [hTB, wkvB], [PSB[4]])
                        vtt, vB = vt[sl]
                        cp(vtt[:], psf(4), [PSB[4]], [vB])
                        dma("sp", vbd[i], vtt[:], ds_st[sl], [vB], [])
                        if pseq is not None and KDBG >= 3:
                            vff, vfB = vf[sl]
                            cp(vff[:], psf(4), [PSB[4], vB] if os.environ.get("VFS") else [PSB[4]], [vfB], eng=os.environ.get("VFE", "act"))
                            r0 = (i - t0) * P
                            if KS3 >= 0:
                                dma("sp", nav[pseq, l, r0:r0 + P, :], vff[:, 0:256], ds_st[2], [vfB], [])
                            if KS3 >= 0 or KS3 == -2:
                                dma("sp", nbv[pseq, l, r0:r0 + P, :], vff[:, 256:384], ds_st[2], [vfB], [])
                                dma("sp", ncv[pseq, l, r0:r0 + P, :], vff[:, 384:512], ds_st[2], [vfB], [])
                            if KS3 < 1:
                                continue
                            for k in range(8):
                                mm(psf(5), hT[:, k, ii * P:(ii + 1) * P], wkv[:, k, 0:512], k == 0, k == 7, [hTB, wkvB], [PSB[5]])
                            kff, kfB = kf[sl]
                            cp(kff[:], psf(5), [PSB[5]], [kfB], eng="act")
                            for hh in range(2 if KS3 >= 2 else 0):
                                seg = kff[:, 256 + hh * 64:256 + (hh + 1) * 64]
                                tt(junk[:], seg, seg, ALU.mult, [kfB], [junkB])
                                S.op("dve", lambda e, hh=hh: e.reduce_sum(ssq[:, hh:hh + 1], junk[:], AX.X), [junkB], [ssqB])
                            if KS3 >= 3:
                                ts(ssq[:], ssq[:], 1.0 / 64, RMS_EPS, ALU.mult, ALU.add, [ssqB], [ssqB])
                                rsqrt_(ssq[:], ssqB)
                            for hh in range(2 if KS3 >= 4 else 0):
                                seg = kff[:, 256 + hh * 64:256 + (hh + 1) * 64]
                                stt(seg, seg, ssq[:, hh:hh + 1], gkb[:], ALU.mult, ALU.mult, [kfB, ssqB, gkbB], [kfB])
                            dma("sp", nak[pseq, l, r0:r0 + P, :], kff[:, 0:256], ds_st[3], [kfB], [])
                            dma("sp", nbk[pseq, l, r0:r0 + P, :], kff[:, 256:384], ds_st[3], [kfB], [])
                            dma("sp", nck[pseq, l, r0:r0 + P, :], kff[:, 384:512], ds_st[3], [kfB], [])
                    if KDBG < 4:
                        continue
                    dma("sp", hTd.rearrange("k p t -> p k t")[:, :, tok0:tok0 + N], hT[:, :, :N], ds_st[2], [hTB], [])
                    if KDBG < 5:
                        continue
                    for c in range(4):
                        pb = c % 2
                        for k in range(8):
                            mm(psf(pb, N), wkv[:, k, c * P:(c + 1) * P], hT[:, k, :N], k == 0, k == 7, [wkvB, hTB], [PSB[pb]])
                        kot, koB = ko[pb]
                        rk = None if not rope else (32 if c < 2 else 64)
                        qk_post(pb, N, c == 2, rk, 1.0, gkc[:, 0:1], gkcB, kot[:, :N], koB)
                        dma("sp", ktd[c, :, tok0:tok0 + N], kot[:, :N], ds_st[pb], [koB], [])
            S.barrier()
        if stop_after == "kpass" and l == depth_run - 1:
            break

        with contextlib.ExitStack() as st:
            KT, KTB = T(st, "KT", [P, 2, NT_S * P + PAST], BF16)
            VBt, VBB = T(st, "VBt", [P, NT_S + 4, 4, 128], BF16)
            wq, wqB = T(st, "wq", [P, 8, 768], BF16)
            hTa, hTaB = T(st, "hTa", [P, 8, 512], BF16)
            QT, QTB = T(st, "QT", [P, 6, 512], BF16)
            mixT, mixB = T(st, "mixT", [P, 6, 512], BF16)
            PT = [T(st, "PT%d" % i, [P, 512], BF16) for i in range(3)]
            tab = [T(st, "atab%d" % i, [P, 512]) for i in range(4)]
            q32, q32B = T(st, "aq32", [P, 512])
            sq, sqB = T(st, "asq", [P, 512])
            r1, r1B = T(st, "ar1", [P, 512])
            t1, t1B = T(st, "at1", [P, 512])
            t2, t2B = T(st, "at2", [P, 512])
            gqc, gqcB = T(st, "gqc", [P, 1])
            ck, ckB = T(st, "ck", [P, 256])
            ckb, ckbB = T(st, "ckb", [P, 256], BF16)
            rc, rcB = T(st, "rc", [P, 512])
            tmpd, tmpdB = T(st, "tmpd", [P, 512])
            o1n, o1B = T(st, "o1n", [64, 512])
            oa, oaB = T(st, "oa", [64, 512])
            osq, osqB = T(st, "osq", [64, 512])
            ors, orsB = T(st, "ors", [64, 512])
            lamt, lamB = T(st, "lamt", [P, 128])
            lam2, lam2B = T(st, "lam2", [P, 2])
            neglam, nlB = T(st, "neglam", [P, 1])
            gsub, gsubB = T(st, "gsub", [P, 1])
            esink, esB = T(st, "esink", [P, 6])

            dma("sp", lamt[:], a_lambda[l:l + 1, :].to_broadcast([P, 128]), None, [], [lamB])
            for pr in range(2):
                tt(lamt[:, pr * 64:pr * 64 + 32], lamt[:, pr * 64:pr * 64 + 32], lamt[:, pr * 64 + 32:pr * 64 + 64],
                   ALU.mult, [lamB], [lamB])
                S.op("dve", lambda e, pr=pr: e.reduce_sum(lam2[:, pr:pr + 1], lamt[:, pr * 64:pr * 64 + 32], AX.X), [lamB], [lam2B])
            act(lam2[:], lam2[:], AF.Exp, [lam2B], [lam2B])
            stt(neglam[:], lam2[:, 1:2], -float(lam0), lam2[:, 0:1], ALU.add, ALU.subtract, [lam2B], [nlB])
            for hh in range(2):
                dma("sp", gsub[hh * 64:(hh + 1) * 64, :], a_subln_g[l, :].rearrange("(d o) -> d o", o=1), None, [], [gsubB])
                dma("sp", gqc[hh * 64:(hh + 1) * 64, :], b_q_norm_g[l, :].rearrange("(d o) -> d o", o=1), None, [], [gqcB])
            ts(gsub[:], gsub[:], float(1.0 - lam0), None, ALU.mult, None, [gsubB], [gsubB])
            ts(gqc[:], gqc[:], float(64 ** -0.5), None, ALU.mult, None, [gqcB], [gqcB])
            dma("sp", esink[:], c_sink[l:l + 1, :].to_broadcast([P, 6]), None, [], [esB])
            act(esink[:], esink[:], AF.Exp, [esB], [esB])

            def qk_post_a(psb_i, N, norm, rope_kind, scale, out_ap, outB):
                src = psf(psb_i, N)
                srcB = PSB[psb_i]
                if norm:
                    act(sq[:, :N], src, AF.Square, [srcB], [sqB])
                    mm(psf(2, N), csb["blk64_f"][:], sq[:, :N], True, True, [sqB, cbuf], [PSB[2]])
                    ts(r1[:, :N], psf(2, N), 1.0 / 64, RMS_EPS, ALU.mult, ALU.add, [PSB[2]], [r1B])
                    rsqrt_(r1[:, :N], r1B)
                    ts(r1[:, :N], r1[:, :N], gqc[:, 0:1], None, ALU.mult, None, [r1B, gqcB], [r1B])
                    if rope_kind is None:
                        tt(out_ap, src, r1[:, :N], ALU.mult, [srcB, r1B], [outB])
                        return
                    tt(q32[:, :N], src, r1[:, :N], ALU.mult, [srcB, r1B], [q32B])
                else:
                    if rope_kind is None:
                        act(out_ap, src, AF.Copy, [srcB], [outB], scale=float(scale))
                        return
                    act(q32[:, :N], src, AF.Copy, [srcB], [q32B], scale=float(scale))
                rotm = csb["rot64_f"] if rope_kind == 64 else csb["rot32_f"]
                ct, ctB = tab[0] if rope_kind == 64 else tab[2]
                sn, snB = tab[1] if rope_kind == 64 else tab[3]
                mm(psf(3, N), rotm[:], q32[:, :N], True, True, [q32B, cbuf], [PSB[3]])
                tt(t1[:, :N], q32[:, :N], ct[:, :N], ALU.mult, [q32B, ctB], [t1B])
                tt(t2[:, :N], psf(3, N), sn[:, :N], ALU.mult, [PSB[3], snB], [t2B])
                tt(out_ap, t1[:, :N], t2[:, :N], ALU.add, [t1B, t2B], [outB])

            st_ctr = [0]

            def head(N, nkc, qci, krow0, kch, nrows, vh, dst_chunk, dst_base, kind, hidx, win):
                ob = 4 + (st_ctr[0] % 2)
                st_ctr[0] += 1
                chunks = win if win is not None else [(kc, None) for kc in range(nkc)]
                for ci, (kc, mr) in enumerate(chunks):
                    sb = 6 + (ci % 2)
                    mm(psf(sb, N), KT[krow0:krow0 + nrows, kch, kc * P:(kc + 1) * P], QT[krow0:krow0 + nrows, qci, :N],
                       True, True, [KTB, QTB], [PSB[sb]])
                    ptt, ptB = PT[ci % 3]
                    act(ptt[:, :N], psf(sb, N), AF.Exp, [PSB[sb]], [ptB])
                    if mr is not None:
                        tt(ptt[:, :N], ptt[:, :N], wmask[:, mr, :N], ALU.mult, [ptB, cbuf], [ptB])
                    mm(psf(ob, N), VBt[:, kc, vh, :], ptt[:, :N], ci == 0, ci == len(chunks) - 1,
                       [VBB, ptB], [PSB[ob]])
                oB = PSB[ob]
                if kind == "C":
                    ts(tmpd[64:128, :N], psf(ob, N)[64:128, :], esink[64:128, hidx:hidx + 1], None, ALU.add, None, [oB, esB], [tmpdB])
                    S.op("dve", lambda e: e.reciprocal(rc[0:64, :N], tmpd[64:128, :N]), [tmpdB], [rcB])
                else:
                    S.op("dve", lambda e: e.reciprocal(rc[0:64, :N], psf(ob, N)[64:128, :]), [oB], [rcB])
                if kind in ("B", "C"):
                    tt(mixT[dst_base:dst_base + 64, dst_chunk, :N], psf(ob, N)[0:64, :], rc[0:64, :N], ALU.mult, [oB, rcB], [mixB])
                elif kind == "A1":
                    tt(o1n[:, :N], psf(ob, N)[0:64, :], rc[0:64, :N], ALU.mult, [oB, rcB], [o1B])
                else:
                    tt(oa[:, :N], psf(ob, N)[0:64, :], rc[0:64, :N], ALU.mult, [oB, rcB], [oaB])
                    stt(oa[:, :N], oa[:, :N], neglam[0:64, 0:1], o1n[:, :N], ALU.mult, ALU.add, [oaB, nlB, o1B], [oaB])
                    act(osq[:, :N], oa[:, :N], AF.Square, [oaB], [osqB])
                    mm(psf(2, N)[0:64, :], csb["ones_f"][0:64, 0:64], osq[:, :N], True, True, [osqB, cbuf], [PSB[2]])
                    ts(ors[:, :N], psf(2, N)[0:64, :], 1.0 / 64, RMS_EPS, ALU.mult, ALU.add, [PSB[2]], [orsB])
                    rsqrt_(ors[:, :N], orsB)
                    stt(mixT[dst_base:dst_base + 64, dst_chunk, :N], oa[:, :N], gsub[0:64, 0:1], ors[:, :N], ALU.mult, ALU.mult,
                        [oaB, gsubB, orsB], [mixB])

            for (cj, t0, ntl, rope, has_cache, pseq) in UNITS:
                nkc_new = ntl
                nkc = ntl + (4 if has_cache else 0)
                tokU = t0 * P
                for tg in ("A", "BC"):
                    kch0 = 0 if tg == "A" else 2
                    vcol0 = 0 if tg == "A" else 256
                    dma("sp", KT[:, :, 0:ntl * P], ktd[kch0:kch0 + 2, :, tokU:tokU + ntl * P].rearrange("c p t -> p c t"), None, [], [KTB])
                    for vh_ in range(4):
                        dma("sp", VBt[:, 0:ntl, vh_, 0:64],
                            vbd[t0:t0 + ntl, :, vcol0 + vh_ * 64:vcol0 + (vh_ + 1) * 64].rearrange("c p f -> p c f"), None, [], [VBB])
                    S.op("dve", lambda e: e.memset(VBt[:, :, :, 64:128], 1.0), [], [VBB])
                    if has_cache:
                        for vh_ in range(4):
                            if tg == "A":
                                src_v = cav[l, :, vh_ * 64:(vh_ + 1) * 64]
                            elif vh_ < 2:
                                src_v = cbv[l, :, vh_ * 64:(vh_ + 1) * 64]
                            else:
                                src_v = ccv[l, :, (vh_ - 2) * 64:(vh_ - 1) * 64]
                            dma("pool", VBt[:, ntl:ntl + 4, vh_, 0:64], src_v.rearrange("(c p) f -> p c f", p=P), None, [], [VBB])
                        for ctile in range(4):
                            if tg == "A":
                                dma("sp", ck[:], cak[l, ctile * P:(ctile + 1) * P, :], None, [], [ckB])
                            else:
                                dma("sp", ck[:, 0:128], cbk[l, ctile * P:(ctile + 1) * P, :], None, [], [ckB])
                                dma("sp", ck[:, 128:256], cck[l, ctile * P:(ctile + 1) * P, :], None, [], [ckB])
                            cp(ckb[:], ck[:], [ckB], [ckbB])
                            for c2 in range(2):
                                tp(psbf(0)[:, c2 * P:(c2 + 1) * P], ckb[:, c2 * P:(c2 + 1) * P], ident_bf[:], [ckbB, cbuf], [PSB[0]])
                            cp(KT[:, :, (ntl + ctile) * P:(ntl + ctile + 1) * P], psbf(0)[:, 0:2 * P].rearrange("p (c t) -> p c t", t=P),
                               [PSB[0]], [KTB], eng="act")
                    if tg == "A":
                        dma("pool", wq[:, :, 0:256], w_in[l, :, QA:QA + 256].rearrange("(k p) n -> p k n", p=P), None, [], [wqB])
                    else:
                        for hq_ in range(6):
                            o_ = ((hq_ % 3) * 2 + hq_ // 3) * 64
                            dma("pool", wq[:, :, o_:o_ + 64], w_in[l, :, QB + hq_ * 64:QB + (hq_ + 1) * 64].rearrange("(k p) n -> p k n", p=P),
                                None, [], [wqB])
                            dma("pool", wq[:, :, 384 + o_:384 + o_ + 64],
                                w_in[l, :, QC + hq_ * 64:QC + (hq_ + 1) * 64].rearrange("(k p) n -> p k n", p=P), None, [], [wqB])
                    ngrp = (ntl + 3) // 4
                    for g in range(ngrp):
                        tiles = list(range(t0 + 4 * g, min(t0 + 4 * g + 4, t0 + ntl)))
                        N = P * len(tiles)
                        tok0 = tiles[0] * P
                        pos0 = (tiles[0] - t0) * P
                        dma("sp", hTa[:, :, :N], hTd.rearrange("k p t -> p k t")[:, :, tok0:tok0 + N], None, [], [hTaB])
                        if rope:
                            for ti, nm in enumerate(("cos64", "sin64", "cos32", "sin32")):
                                dma("sp", tab[ti][0][:, :N], cin[nm][:, pos0:pos0 + N], None, [], [tab[ti][1]])
                        nq = 2 if tg == "A" else 6
                        for qi in range(nq):
                            pb = qi % 2
                            if tg == "A":
                                lw = lambda k, qi=qi: wq[:, k, qi * P:(qi + 1) * P]
                                norm, rk, sc = False, (32 if rope else None), 32 ** -0.5
                            else:
                                m3 = qi % 3
                                base = 0 if qi < 3 else 384
                                lw = lambda k, m3=m3, base=base: wq[:, k, base + m3 * P:base + (m3 + 1) * P]
                                norm, rk, sc = (qi < 3), (64 if rope else None), 64 ** -0.5
                            for k in range(8):
                                mm(psf(pb, N), lw(k), hTa[:, k, :N], k == 0, k == 7, [wqB, hTaB], [PSB[pb]])
                            qk_post_a(pb, N, norm, rk, sc, QT[:, qi, :N], QTB)
                            if tg == "A":
                                for m in range(2):
                                    ts(QT[:, 2 + 2 * m + qi, :N], QT[:, qi, :N], csb["mcol"][:, m:m + 1], None, ALU.mult, None,
                                       [QTB, cbuf], [QTB])
                        if tg == "A":
                            for h in range(4):
                                for m in range(2):
                                    head(N, nkc, 2 + 2 * m + h // 2, (h % 2) * 64, h // 2, 64, h, h // 2, (h % 2) * 64,
                                         "A1" if m == 0 else "A2", h, None)
                        else:
                            for hq in range(6):
                                gq, m3 = hq // 3, hq % 3
                                h16 = 4 + hq
                                head(N, nkc, m3, gq * 64, 0, 64, gq, (h16 // 2) - 2, (h16 % 2) * 64, "B", hq, None)
                            for hq in range(6):
                                gq, m3 = hq // 3, hq % 3
                                h16 = 10 + hq
                                win = None
                                if rope:
                                    i0 = tiles[0] - t0
                                    win = [(i0 - 1 + r, r) for r in range(6) if 0 <= i0 - 1 + r < ntl]
                                    win += [(ntl + c4, None) for c4 in range(4)]
                                head(N, nkc, 3 + m3, gq * 64, 1, 64, 2 + gq, (h16 // 2) - 2, (h16 % 2) * 64, "C", hq, win)
                        nmc = 2 if tg == "A" else 6
                        mc0 = 0 if tg == "A" else 2
                        dma("sp", mixTd[mc0:mc0 + nmc, :, tok0:tok0 + N].rearrange("c p t -> p c t"), mixT[:, 0:nmc, :N], None, [mixB], [])
            S.barrier()
        if stop_after == "apass" and l == depth_run - 1:
            break

        with contextlib.ExitStack() as st:
            wo, woB = T(st, "wo", [P, 8, D], BF16)
            wr, wrB = T(st, "wr", [P, 8, NE])
            brb, brbB = T(st, "brb", [P, NE])
            g1t = [T(st, "g1t%d" % j, [P, D]) for j in range(2)]
            sc2 = [T(st, "sc2%d" % j, [P, D]) for j in range(2)]
            sh2 = [T(st, "sh2%d" % j, [P, D]) for j in range(2)]
            lg_, lgB_ = T(st, "ln1g", [P, D])
            lb_, lbB_ = T(st, "ln1b", [P, D])
            gbB = Buf("ln1gb")
            mt = [T(st, "mt%d" % i, [P, 8, P], BF16) for i in range(2)]
            xt = [T(st, "txt%d" % i, [P, D]) for i in range(2)]
            xa = [T(st, "xa%d" % i, [P, D]) for i in range(2)]
            x1 = [T(st, "x1%d" % i, [P, D]) for i in range(2)]
            h2 = [T(st, "h2%d" % i, [P, D]) for i in range(2)]
            h2b = [T(st, "h2b%d" % i, [P, D], BF16) for i in range(2)]
            h2T, h2TB = T(st, "h2T", [P, 8, P])
            pool = {"t_stats": T(st, "t_stats", [P, 2, 6]), "t_mv": T(st, "t_mv", [P, 2])}
            mask, maskB = T(st, "mask", [P, NE])
            negm, negmB = T(st, "negm", [P, 1])

            dma("pool", wo[:], w_out[l].rearrange("(k p) n -> p k n", p=P), None, [], [woB])
            dma("sp", wr[:], w_router[l].rearrange("(k p) n -> p k n", p=P), None, [], [wrB])
            dma("sp", brb[:], b_router[l:l + 1, :].to_broadcast([P, NE]), None, [], [brbB])
            for j in range(2):
                dma("sp", g1t[j][0][:], modd[l, j:j + 1, 2 * D:3 * D].to_broadcast([P, D]), None, [], [g1t[j][1]])
                dma("sp", sh2[j][0][:], modd[l, j:j + 1, 3 * D:4 * D].to_broadcast([P, D]), None, [], [sh2[j][1]])
                dma("sp", sc2[j][0][:], modd[l, j:j + 1, 4 * D:5 * D].to_broadcast([P, D]), None, [], [sc2[j][1]])
            dma("sp", lg_[:], ln1_g[l:l + 1, :].to_broadcast([P, D]), None, [], [lgB_])
            dma("sp", lb_[:], ln1_b[l:l + 1, :].to_broadcast([P, D]), None, [], [lbB_])
            S.op("dve", lambda e: e.memset(tot[:], 0.0), [], [totB])
            for i in range(NT):
                sl = i % 2
                cj = 0 if i < NT_P else 1
                mtt, mtB = mt[sl]
                xtt, xB = xt[sl]
                xat, xaB = xa[sl]
                x1t, x1B = x1[sl]
                h2t, h2B = h2[sl]
                h2bt, h2bB = h2b[sl]
                dma("sp", mtt[:], mixTd[:, :, i * P:(i + 1) * P].rearrange("c p t -> p c t"), None, [], [mtB])
                dma("sp", xtt[:], x_src(l, i), None, [], [xB])
                for nh in range(2):
                    for c in range(8):
                        mm(psf(nh), mtt[:, c, :], wo[:, c, nh * 512:(nh + 1) * 512], c == 0, c == 7, [mtB, woB], [PSB[nh]])
                for nh in range(2):
                    tt(xat[:, nh * 512:(nh + 1) * 512], psf(nh), g1t[cj][0][:, nh * 512:(nh + 1) * 512], ALU.mult,
                       [PSB[nh], g1t[cj][1]], [xaB])
                stt(xat[:], xtt[:], float(ALPHA), xat[:], ALU.mult, ALU.add, [xB, xaB], [xaB])
                layer_norm(pool, xat, xaB, lg_, lb_, lgB_, lbB_, x1t, x1B, "t_")
                dma("sp", x1d[i * P:(i + 1) * P, :], x1t[:], None, [x1B], [])
                tt(h2t[:], x1t[:], sc2[cj][0][:], ALU.mult, [x1B, sc2[cj][1]], [h2B])
                tt(h2t[:], h2t[:], sh2[cj][0][:], ALU.add, [h2B, sh2[cj][1]], [h2B])
                cp(h2bt[:], h2t[:], [h2B], [h2bB], eng="act")
                dma("sp", h2d[i * P:(i + 1) * P, :], h2bt[:], None, [h2bB], [])
                for k in range(8):
                    tp(ps_all[:, 2 * 512 + k * P: 2 * 512 + (k + 1) * P], h2t[:, k * P:(k + 1) * P], ident_f[:], [h2B, cbuf], [PSB[2 + k // 4]])
                cp(h2T[:, 0:4, :], psf(2).rearrange("p (k t) -> p k t", t=P), [PSB[2]], [h2TB], eng="act")
                cp(h2T[:, 4:8, :], psf(3).rearrange("p (k t) -> p k t", t=P), [PSB[3]], [h2TB], eng="act")
                for k in range(8):
                    mm(psf(4, NE), h2T[:, k, :], wr[:, k, :], k == 0, k == 7, [h2TB, wrB], [PSB[4]])
                tt(lgall[:, i, :], psf(4, NE), brb[:], ALU.add, [PSB[4], brbB], [lgB])
                S.op("dve", lambda e, i=i: e.max(top8all[:, i, :], lgall[:, i, :]), [lgB], [t8B])
                ts(mask[:], lgall[:, i, :], top8all[:, i, 3:4], None, ALU.is_ge, None, [lgB, t8B], [maskB])
                mm(psf(5, NE), csb["tri_f"][:], mask[:], True, True, [maskB, cbuf], [PSB[5]])
                mm(psf(6, NE), csb["ones_f"][:], mask[:], True, True, [maskB, cbuf], [PSB[6]])
                tt(rankall[:, i, :], psf(5, NE), tot[:], ALU.add, [PSB[5], totB], [rkB])
                tt(tot[:], tot[:], psf(6, NE), ALU.add, [totB, PSB[6]], [totB])
            S.barrier()
        if stop_after == "tpass" and l == depth_run - 1:
            break

        with contextlib.ExitStack() as st:
            cnt, cntB = T(st, "cnt", [NE, 1])
            cnti, cntiB = T(st, "cnti", [NE, 1], I32)
            padf, padfB = T(st, "padf", [NE, 1])
            padrep, prB = T(st, "padrep", [NE, P])
            pst, pstB = T(st, "pst", [P, NE])
            pcol, pcolB = T(st, "pcol", [NE, 2])
            cmpa, cmpaB = T(st, "cmpa", [NE, NBLK])
            cmpb, cmpbB = T(st, "cmpb", [NE, NBLK])
            bef, befB = T(st, "bef", [P, NBLK])
            kpl, kplB = T(st, "kpl", [P, 8])
            widf, widfB = T(st, "widf", [P, NBLK, 8])
            dall, dallB = T(st, "dall", [P, NT, NE])
            oh, ohB2 = T(st, "oh", [P, NT, NE])
            dk, dkB = T(st, "dk", [P, NT, TOPK])
            e4, e4B = T(st, "e4", [P, NT, TOPK])
            s4, s4B = T(st, "s4", [P, NT])
            mm(psf(0, 1)[0:NE, :], tot[:, :], ident_f[:, 0:1], True, True, [totB, cbuf], [PSB[0]])
            cp(cnti[:], psf(0, 1)[0:NE, :], [PSB[0]], [cntiB])
            ts(cnti[:], cnti[:], BLK - 1, None, ALU.add, None, [cntiB], [cntiB])
            ts(cnti[:], cnti[:], 8, None, ALU.arith_shift_right, None, [cntiB], [cntiB])
            ts(cnti[:], cnti[:], 8, None, ALU.logical_shift_left, None, [cntiB], [cntiB])
            cp(padf[:], cnti[:], [cntiB], [padfB])
            cp(padrep[:], padf[:, 0:1].to_broadcast([NE, P]), [padfB], [prB])
            mm(psf(1, NE), padrep[:], csb["tri32_f"][:], True, True, [prB, cbuf], [PSB[1]])
            cp(pst[:], psf(1, NE), [PSB[1]], [pstB])
            mm(psf(2, 1)[0:NE, :], csb["tri32_f"][:], padf[:], True, True, [padfB, cbuf], [PSB[2]])
            mm(psf(3, 1)[0:NE, :], csb["inc32_f"][:], padf[:], True, True, [padfB, cbuf], [PSB[3]])
            cp(pcol[:, 0:1], psf(2, 1)[0:NE, :], [PSB[2]], [pcolB])
            cp(pcol[:, 1:2], psf(3, 1)[0:NE, :], [PSB[3]], [pcolB])
            ts(cmpa[:], csb["blkstart"][:], pcol[:, 0:1], None, ALU.is_ge, None, [pcolB, cbuf], [cmpaB])
            ts(cmpb[:], csb["blkstart"][:], pcol[:, 1:2], None, ALU.is_ge, None, [pcolB, cbuf], [cmpbB])
            tt(OH[:], cmpa[:], cmpb[:], ALU.subtract, [cmpaB, cmpbB], [ohB])
            mm(psf(4, NBLK), csb["ones_f"][0:NE, :], cmpb[:], True, True, [cmpbB, cbuf], [PSB[4]])
            ts(bef[:], psf(4, NBLK), float(NE - 1), 1024.0, ALU.min, ALU.mult, [PSB[4]], [befB])
            ts(kpl[:], csb["kpc"][:], float(l * NE * 1024), None, ALU.add, None, [cbuf], [kplB])
            tt(widf[:], bef[:].rearrange("p (b o) -> p b o", o=1).to_broadcast([P, NBLK, 8]),
               kpl[:].rearrange("p (o k) -> p o k", o=1).to_broadcast([P, NBLK, 8]), ALU.add, [befB, kplB], [widfB])
            cp(widx[:], widf[:], [widfB], [wiB])
            tt(dall[:], rankall[:], pst[:].rearrange("p (o e) -> p o e", o=1).to_broadcast([P, NT, NE]), ALU.add, [rkB, pstB], [dallB])
            for k in range(TOPK):
                tt(oh[:], lgall[:], top8all[:, :, k:k + 1].to_broadcast([P, NT, NE]), ALU.is_equal, [lgB, t8B], [ohB2])
                tt(oh[:], oh[:], dall[:], ALU.mult, [ohB2, dallB], [ohB2])
                S.op("dve", lambda e, k=k: e.reduce_sum(dk[:, :, k], oh[:], AX.X), [ohB2], [dkB])
            cp(desti[:], dk[:], [dkB], [diB])
            tt(e4[:], top8all[:, :, 0:TOPK], top8all[:, :, 0:1].to_broadcast([P, NT, TOPK]), ALU.subtract, [t8B], [e4B])
            act(e4[:], e4[:], AF.Exp, [e4B], [e4B])
            S.op("dve", lambda e: e.reduce_sum(s4[:], e4[:], AX.X), [e4B], [s4B])
            S.op("dve", lambda e: e.reciprocal(s4[:], s4[:]), [s4B], [s4B])
            tt(gk[:], e4[:], s4[:].rearrange("p (n o) -> p n o", o=1).to_broadcast([P, NT, TOPK]), ALU.mult, [e4B, s4B], [gkB])
            S.barrier()

        with contextlib.ExitStack() as st:
            hbs = [T(st, "hbs%d" % i, [P, D], BF16) for i in range(3)]
            for i in range(NT):
                hbt, hbB = hbs[i % 3]
                dma("sp", hbt[:], h2d[i * P:(i + 1) * P, :], None, [], [hbB])
                for k in range(TOPK):
                    S.dma("pool", lambda e, i=i, k=k, hbt=hbt: e.indirect_dma_start(
                        out=mbuf[:, :], out_offset=bass.IndirectOffsetOnAxis(ap=desti[:, i, k:k + 1], axis=0),
                        in_=hbt[:, :], in_offset=None), None, [hbB], [])
            S.barrier()
        if stop_after == "bpass" and l == depth_run - 1:
            break

        with contextlib.ExitStack() as st:
            wgu = [T(st, "wgu%d" % i, [P, 8, 2 * D], BF16) for i in range(2)]
            wdn = [T(st, "wdn%d" % i, [P, 8, D], BF16) for i in range(2)]
            bgu, bguB = T(st, "bgu", [NE, 2 * D], BF16)
            bdn, bdnB = T(st, "bdn", [NE, D], BF16)
            xb = [T(st, "xb%d" % i, [P, 2, D], BF16) for i in range(2)]
            xT, xTB = T(st, "xT", [P, 8, BLK], BF16)
            ohb, ohbB = T(st, "ohb", [NE, BLK], BF16)
            gg, ggB = T(st, "gg", [P, BLK])
            sg, sgB = T(st, "sgm", [P, BLK])
            ll, llB = T(st, "ll", [P, BLK])
            actT, actB = T(st, "actT", [P, 8, BLK], BF16)
            yt = [T(st, "yt%d" % i, [P, 2, D]) for i in range(2)]
            dma("pool", bgu[:], b_gate_up[l], None, [], [bguB])
            dma("pool", bdn[:], b_down[l], None, [], [bdnB])
            wgu_rows = w_gate_up.rearrange("l e r n -> (l e r) n")
            wdn_rows = w_down.rearrange("l e r n -> (l e r) n")
            for b in range(NBLK):
                sl = b % 2
                wgt, wgB = wgu[sl]
                wdt, wdB = wdn[sl]

                for k in range(8):
                    S.dma("pool", lambda e, b=b, k=k, wgt=wgt: e.indirect_dma_start(
                        out=wgt[:, k, :], out_offset=None, in_=wgu_rows[:, :],
                        in_offset=bass.IndirectOffsetOnAxis(ap=widx[:, b, k:k + 1], axis=0)), None, [], [wgB])
                for k in range(8):
                    S.dma("pool", lambda e, b=b, k=k, wdt=wdt: e.indirect_dma_start(
                        out=wdt[:, k, :], out_offset=None, in_=wdn_rows[:, :],
                        in_offset=bass.IndirectOffsetOnAxis(ap=widx[:, b, k:k + 1], axis=0)), None, [], [wdB])
                xbt, xbB = xb[sl]
                dma("sp", xbt[:], mbuf[b * BLK:(b + 1) * BLK, :].rearrange("(j p) d -> p j d", p=P), None, [], [xbB])
                cp(ohb[:], OH[:, b:b + 1].to_broadcast([NE, BLK]), [ohB], [ohbB])
                for j in range(2):
                    for k in range(8):
                        tp(psbf(0, 2).rearrange("p (k t) -> p k t", t=BLK)[:, k, j * P:(j + 1) * P],
                           xbt[:, j, k * P:(k + 1) * P], ident_bf[:], [xbB, cbuf], [PSB[0], PSB[1]])
                cp(xT[:], psbf(0, 2).rearrange("p (k t) -> p k t", t=BLK), [PSB[0], PSB[1]], [xTB], eng="act")
                for j in range(8):
                    pb = 2 + (j % 2)
                    for half in range(2):
                        c0 = half * D + j * P
                        o_ap = psf(pb, BLK, half * BLK)
                        for k in range(8):
                            mm(o_ap, wgt[:, k, c0:c0 + P], xT[:, k, :], k == 0, False, [wgB, xTB], [PSB[pb]])
                        mm(o_ap, bgu[:, c0:c0 + P], ohb[:], False, True, [bguB, ohbB], [PSB[pb]])
                    ts(gg[:], psf(pb, BLK, 0), SW_LIM, None, ALU.min, None, [PSB[pb]], [ggB])
                    act(sg[:], gg[:], AF.Sigmoid, [ggB], [sgB], scale=SW_ALPHA)
                    ts(ll[:], psf(pb, BLK, BLK), SW_LIM, -SW_LIM, ALU.min, ALU.max, [PSB[pb]], [llB])
                    tt(gg[:], gg[:], sg[:], ALU.mult, [ggB, sgB], [ggB])
                    stt(actT[:, j, :], ll[:], 1.0, gg[:], ALU.add, ALU.mult, [llB, ggB], [actB])
                ytt, yB = yt[sl]
                for rt in range(2):
                    for nh in range(2):
                        pb = 4 + ((rt * 2 + nh) % 4)
                        for j in range(8):
                            mm(psf(pb), actT[:, j, rt * P:(rt + 1) * P], wdt[:, j, nh * 512:(nh + 1) * 512], j == 0, False,
                               [actB, wdB], [PSB[pb]])
                        mm(psf(pb), ohb[:, rt * P:(rt + 1) * P], bdn[:, nh * 512:(nh + 1) * 512], False, True, [ohbB, bdnB], [PSB[pb]])
                        cp(ytt[:, rt, nh * 512:(nh + 1) * 512], psf(pb), [PSB[pb]], [yB], eng="act")
                dma("sp", obuf[b * BLK:(b + 1) * BLK, :].rearrange("(j p) d -> p j d", p=P), ytt[:], None, [yB], [])
            S.barrier()
        if stop_after == "cpass" and l == depth_run - 1:
            break

        with contextlib.ExitStack() as st:
            g2t = [T(st, "g2t%d" % j, [P, D]) for j in range(2)]
            lg_, lgB_ = T(st, "ln2g", [P, D])
            lb_, lbB_ = T(st, "ln2b", [P, D])
            yk = [[T(st, "yk%d_%d" % (i, k), [P, D]) for k in range(TOPK)] for i in range(2)]
            x1 = [T(st, "dx1%d" % i, [P, D]) for i in range(2)]
            mo = [T(st, "mo%d" % i, [P, D]) for i in range(2)]
            xo = [T(st, "xo%d" % i, [P, D]) for i in range(2)]
            pool = {"d_stats": T(st, "d_stats", [P, 2, 6]), "d_mv": T(st, "d_mv", [P, 2])}
            for j in range(2):
                dma("sp", g2t[j][0][:], modd[l, j:j + 1, 5 * D:6 * D].to_broadcast([P, D]), None, [], [g2t[j][1]])
            dma("sp", lg_[:], ln2_g[l:l + 1, :].to_broadcast([P, D]), None, [], [lgB_])
            dma("sp", lb_[:], ln2_b[l:l + 1, :].to_broadcast([P, D]), None, [], [lbB_])
            for i in range(NT):
                sl = i % 2
                cj = 0 if i < NT_P else 1
                x1t, x1B = x1[sl]
                mot, moB = mo[sl]
                xot, xoB = xo[sl]
                dma("sp", x1t[:], x1d[i * P:(i + 1) * P, :], None, [], [x1B])
                for k in range(TOPK):
                    ykt, ykB = yk[sl][k]
                    S.dma("pool", lambda e, i=i, k=k, ykt=ykt: e.indirect_dma_start(
                        out=ykt[:, :], out_offset=None, in_=obuf[:, :],
                        in_offset=bass.IndirectOffsetOnAxis(ap=desti[:, i, k:k + 1], axis=0)), None, [], [ykB])
                ts(mot[:], yk[sl][0][0][:], gk[:, i, 0:1], None, ALU.mult, None, [yk[sl][0][1], gkB], [moB])
                for k in range(1, TOPK):
                    stt(mot[:], yk[sl][k][0][:], gk[:, i, k:k + 1], mot[:], ALU.mult, ALU.add, [yk[sl][k][1], gkB, moB], [moB])
                tt(mot[:], mot[:], g2t[cj][0][:], ALU.mult, [moB, g2t[cj][1]], [moB])
                stt(mot[:], x1t[:], float(ALPHA), mot[:], ALU.mult, ALU.add, [x1B, moB], [moB])
                layer_norm(pool, mot, moB, lg_, lb_, lgB_, lbB_, xot, xoB, "d_")
                dma("sp", x_dst(l, i), xot[:], None, [xoB], [])
            S.barrier()

    S.barrier()
    es.close()
    return nc, consts


def _in_maps(inputs, consts, cores, nl=DEPTH):
    f = lambda a: np.ascontiguousarray(np.asarray(a, dtype=np.float32)[:nl])
    maps = []
    shared = {k: f(inputs[k]) for k in ("w_mod", "b_mod", "w_in", "a_subln_g", "b_q_norm_g", "b_k_norm_g", "c_sink",
                                        "w_out", "ln1_g", "ln1_b", "w_router", "b_router", "w_gate_up", "b_gate_up",
                                        "w_down", "b_down", "ln2_g", "ln2_b")}
    shared["a_lambda"] = f(inputs["a_lambda"]).reshape(nl, 128)
    for k, v in consts.items():
        shared["c_" + k] = v
    g32 = lambda a: np.ascontiguousarray(np.asarray(a, dtype=np.float32))
    xpr = g32(inputs["x_prompt"])
    xsa = g32(inputs["x_sample"])
    cc = g32(inputs["c"])
    cctx = g32(inputs["c_ctx"])
    for c in cores:
        m = dict(shared)
        m["xp"] = xpr[2 * c:2 * c + 2].reshape(2 * SEQ, D)
        m["xs"] = xsa[c]
        m["cak"] = f(inputs["cache_a_k"][c]).reshape(nl, PAST, 256)
        m["cav"] = f(inputs["cache_a_v"][c]).reshape(nl, PAST, 256)
        m["cbk"] = f(inputs["cache_b_k"][c]).reshape(nl, PAST, 128)
        m["cbv"] = f(inputs["cache_b_v"][c]).reshape(nl, PAST, 128)
        m["cck"] = f(inputs["cache_c_k"][c]).reshape(nl, PAST, 128)
        m["ccv"] = f(inputs["cache_c_v"][c]).reshape(nl, PAST, 128)
        m["cond2"] = np.ascontiguousarray(np.stack([cctx, cc[c]]))
        maps.append(m)
    return maps


def _run(inputs, cores, depth_run=DEPTH, stop_after=None, trace=False):
    nc, consts = build(depth_run, stop_after)
    maps = _in_maps(inputs, consts, cores, depth_run)
    res = run_bass_kernel_spmd(nc, maps, core_ids=list(range(len(cores))), trace=trace)
    return res


def kernel(**inputs):
    cores = list(range(NCORES))
    res = _run(inputs, cores)
    R = res.results
    g = lambda k: [np.asarray(r[k], dtype=np.float32) for r in R]
    y_prompt = np.concatenate([a.reshape(2, SEQ, D) for a in g("yp")], axis=0)
    y_sample = np.stack(g("ys"), axis=0)
    outs = [y_prompt, y_sample]
    for k, hh, dd in (("nak", 4, 64), ("nav", 4, 64), ("nbk", 2, 64), ("nbv", 2, 64), ("nck", 2, 64), ("ncv", 2, 64)):
        outs.append(np.concatenate([a.reshape(2, DEPTH, SEQ, hh, dd) for a in g(k)], axis=0))
    return tuple(outs)
```
